# Optimizing a Trainium2 kernel written in Bass

```python
import math
import jax, jax.numpy as jnp
from jax import lax
import numpy as np

D_MODEL = 2048
BATCH = 2
SEQ = 4096
DEPTH = 2

RET_HEADS = 4
RET_QK_DIM = 64
RET_V_DIM = 128
RET_WIDTH = RET_HEADS * RET_V_DIM
RET_CHUNK = 128
RET_COLS = 2 * RET_HEADS * RET_QK_DIM + 2 * RET_WIDTH

RWKV_HEADS = 8
RWKV_HEAD_DIM = 64
RWKV_WIDTH = RWKV_HEADS * RWKV_HEAD_DIM
RWKV_DECAY_RANK = 96
RWKV_A_RANK = 96
RWKV_GATE_RANK = 256
RWKV_GN_EPS = 64e-5
RWKV_COLS = 3 * RWKV_WIDTH + RWKV_DECAY_RANK + RWKV_A_RANK + RWKV_GATE_RANK

MOBA_HEADS = 8
MOBA_HEAD_DIM = 128
MOBA_WIDTH = MOBA_HEADS * MOBA_HEAD_DIM
MOBA_BLOCK = 256
MOBA_TOPK = 3
MOBA_Q_CHUNK = 32
MOBA_COLS = 3 * MOBA_WIDTH
NEG_INF = -1e30

MIX_WIDTH = RET_WIDTH + RWKV_WIDTH + MOBA_WIDTH
IN_COLS = RET_COLS + RWKV_COLS + MOBA_COLS

D_FF = 5632
CONV_WIDTH = 3
NORM_EPS = 1e-6

kernel_name = "hybrid_retention_rwkv7_moba_convffn_trunk"


def rms_norm(x, g):
    xf = x.astype(jnp.float32)
    y = xf * lax.rsqrt(jnp.mean(xf * xf, axis=-1, keepdims=True) + NORM_EPS)
    return (y * g.astype(jnp.float32)).astype(x.dtype)


def token_shift(x):
    return jnp.pad(x, ((0, 0), (1, 0), (0, 0)))[:, :-1]


def retention_chunkwise(q, k, v):
    B, S, H, Dk = q.shape
    Dv = v.shape[-1]
    C = RET_CHUNK
    N = S // C
    f32 = jnp.float32
    log_g = jnp.log1p(-jnp.exp2(-5.0 - jnp.arange(H, dtype=f32)))

    def chunks(a, d):
        return a.astype(f32).reshape(B, N, C, H, d).transpose(1, 0, 3, 2, 4)

    qc_all = chunks(q, Dk)
    kc_all = chunks(k, Dk) * (Dk ** -0.5)
    vc_all = chunks(v, Dv)
    idx = jnp.arange(C, dtype=f32)
    diff = idx[:, None] - idx[None, :]
    inner_decay = jnp.where(diff >= 0, jnp.exp(log_g[:, None, None] * jnp.maximum(diff, 0.0)), 0.0)
    q_decay = jnp.exp(log_g[:, None] * (idx + 1.0))[..., None]
    k_decay = jnp.exp(log_g[:, None] * (C - 1.0 - idx))[..., None]
    chunk_decay = jnp.exp(log_g * C)[:, None, None]

    def step(R, inp):
        qc, kc, vc = inp
        inner = jnp.einsum('bhid,bhjd->bhij', qc, kc) * inner_decay
        o = jnp.einsum('bhij,bhjv->bhiv', inner, vc) + jnp.einsum('bhid,bhdv->bhiv', qc, R) * q_decay
        R = R * chunk_decay + jnp.einsum('bhjd,bhjv->bhdv', kc * k_decay, vc)
        return R, o

    R0 = jnp.zeros((B, H, Dk, Dv), f32)
    _, o = lax.scan(step, R0, (qc_all, kc_all, vc_all))
    return o.transpose(1, 0, 3, 2, 4).reshape(B, S, H, Dv)


def rwkv7_time_mix(r, k, v, wd, wa, wg, w0, w_up, a0, a_up, g_up, k_k, k_a, r_k, ln_w, ln_b):
    B, S, C = r.shape
    H, N = RWKV_HEADS, RWKV_HEAD_DIM
    f32 = jnp.float32
    r, k, v = r.astype(f32), k.astype(f32), v.astype(f32)
    w_log = -jax.nn.softplus(-(w0 + jnp.tanh(wd.astype(f32)) @ w_up)) - 0.5
    decay = jnp.exp(-jnp.exp(w_log))
    a = jax.nn.sigmoid(a0 + wa.astype(f32) @ a_up)
    g = jax.nn.sigmoid(wg.astype(f32)) @ g_up
    kk = (k * k_k).reshape(B, S, H, N)
    kk = kk / jnp.maximum(jnp.sqrt(jnp.sum(kk * kk, axis=-1, keepdims=True)), 1e-12)
    k = k * (1.0 + (a - 1.0) * k_a)

    def heads_tm(z):
        return jnp.moveaxis(z.reshape(B, S, H, N), 1, 0)

    def step(state, inp):
        r_t, w_t, k_t, v_t, kk_t, a_t = inp
        sa = jnp.einsum('bhij,bhj->bhi', state, -kk_t)
        state = (state * w_t[:, :, None, :]
                 + sa[..., None] * (kk_t * a_t)[:, :, None, :]
                 + v_t[..., None] * k_t[:, :, None, :])
        y = jnp.einsum('bhij,bhj->bhi', state, r_t)
        return state, y

    s0 = jnp.zeros((B, H, N, N), f32)
    _, y = lax.scan(step, s0, (heads_tm(r), heads_tm(decay), heads_tm(k), heads_tm(v),
                               jnp.moveaxis(kk, 1, 0), heads_tm(a)))
    y = jnp.moveaxis(y, 0, 1)
    mean = jnp.mean(y, axis=-1, keepdims=True)
    var = jnp.mean((y - mean) ** 2, axis=-1, keepdims=True)
    y = ((y - mean) * lax.rsqrt(var + RWKV_GN_EPS)).reshape(B, S, C) * ln_w + ln_b
    rh, kh, vh = r.reshape(B, S, H, N), k.reshape(B, S, H, N), v.reshape(B, S, H, N)
    bonus = (jnp.sum(rh * kh * r_k, axis=-1, keepdims=True) * vh).reshape(B, S, C)
    return (y + bonus) * g


def moba_attention(q, k, v):
    B, S, H, D = q.shape
    L = MOBA_BLOCK
    NB = -(-S // L)
    P = NB * L
    topk = min(MOBA_TOPK, NB)
    QC = MOBA_Q_CHUNK
    f32 = jnp.float32
    slopes = jnp.exp2(-8.0 * jnp.arange(1, H + 1, dtype=f32) / H)
    qh = q.astype(f32).transpose(0, 2, 1, 3) * (D ** -0.5)
    pad = ((0, 0), (0, 0), (0, P - S), (0, 0))
    kh = jnp.pad(k.astype(f32).transpose(0, 2, 1, 3), pad)
    vh = jnp.pad(v.astype(f32).transpose(0, 2, 1, 3), pad)
    kb = kh.reshape(B, H, NB, L, D)
    vb = vh.reshape(B, H, NB, L, D)
    k_mean = jnp.mean(kb, axis=3)
    gate = jnp.einsum('bhsd,bhnd->bhsn', qh, k_mean)
    q_block = jnp.arange(S) // L
    fully_past = jnp.arange(NB)[None, :] < q_block[:, None]
    gate = jnp.where(fully_past, gate, -jnp.inf)
    _, sel = lax.top_k(gate, topk)
    b_ix = jnp.arange(B)[:, None, None, None]
    h_ix = jnp.arange(H)[None, :, None, None]
    offs = jnp.arange(L)

    def chunk(ci):
        q0 = ci * QC
        qc = lax.dynamic_slice_in_dim(qh, q0, QC, axis=2)
        sc = lax.dynamic_slice_in_dim(sel, q0, QC, axis=2)
        t = q0 + jnp.arange(QC)
        bq = q0 // L
        kg = kb[b_ix, h_ix, sc]
        vg = vb[b_ix, h_ix, sc]
        s_sel = jnp.einsum('bhqd,bhqnld->bhqnl', qc, kg)
        dist_sel = (t[:, None, None] - (sc[..., None] * L + offs)).astype(f32)
        s_sel = jnp.where((sc < bq)[..., None],
                          s_sel - slopes[:, None, None, None] * dist_sel, NEG_INF)
        ko = lax.dynamic_slice_in_dim(kh, bq * L, L, axis=2)
        vo = lax.dynamic_slice_in_dim(vh, bq * L, L, axis=2)
        s_own = jnp.einsum('bhqd,bhld->bhql', qc, ko)
        dist_own = t[:, None] - (bq * L + offs)[None, :]
        s_own = jnp.where(dist_own >= 0,
                          s_own - slopes[:, None, None] * dist_own.astype(f32), NEG_INF)
        scores = jnp.concatenate([s_sel.reshape(B, H, QC, topk * L), s_own], axis=-1)
        p = jax.nn.softmax(scores, axis=-1)
        p_sel = p[..., :topk * L].reshape(B, H, QC, topk, L)
        p_own = p[..., topk * L:]
        return (jnp.einsum('bhqnl,bhqnld->bhqd', p_sel, vg)
                + jnp.einsum('bhql,bhld->bhqd', p_own, vo))

    out = lax.map(chunk, jnp.arange(S // QC))
    return out.transpose(1, 0, 3, 2, 4).reshape(B, S, H * D)


def hybrid_mixer(h, w_in, w_out, mu, w0, w_up, a0, a_up, g_up, k_k, k_a, r_k, ln_w, ln_b):
    B, S, _ = h.shape
    proj = h @ w_in
    o0 = 0
    qk = RET_HEADS * RET_QK_DIM
    rq = proj[..., o0:o0 + qk].reshape(B, S, RET_HEADS, RET_QK_DIM)
    rk = proj[..., o0 + qk:o0 + 2 * qk].reshape(B, S, RET_HEADS, RET_QK_DIM)
    rv = proj[..., o0 + 2 * qk:o0 + 2 * qk + RET_WIDTH].reshape(B, S, RET_HEADS, RET_V_DIM)
    rg = proj[..., o0 + 2 * qk + RET_WIDTH:o0 + RET_COLS].astype(jnp.float32)
    ro = retention_chunkwise(rq, rk, rv)
    ro = ro * lax.rsqrt(jnp.mean(ro * ro, axis=-1, keepdims=True) + NORM_EPS)
    ret_out = ro.reshape(B, S, RET_WIDTH) * jax.nn.silu(rg)
    rw = proj[..., RET_COLS:RET_COLS + RWKV_COLS]
    rw = rw + (token_shift(rw) - rw) * mu
    c1 = RWKV_WIDTH
    c2 = 2 * RWKV_WIDTH
    c3 = 3 * RWKV_WIDTH
    c4 = c3 + RWKV_DECAY_RANK
    c5 = c4 + RWKV_A_RANK
    rwkv_out = rwkv7_time_mix(rw[..., :c1], rw[..., c1:c2], rw[..., c2:c3],
                              rw[..., c3:c4], rw[..., c4:c5], rw[..., c5:],
                              w0, w_up, a0, a_up, g_up, k_k, k_a, r_k, ln_w, ln_b)
    m0 = RET_COLS + RWKV_COLS
    mq = proj[..., m0:m0 + MOBA_WIDTH].reshape(B, S, MOBA_HEADS, MOBA_HEAD_DIM)
    mk = proj[..., m0 + MOBA_WIDTH:m0 + 2 * MOBA_WIDTH].reshape(B, S, MOBA_HEADS, MOBA_HEAD_DIM)
    mv = proj[..., m0 + 2 * MOBA_WIDTH:m0 + 3 * MOBA_WIDTH].reshape(B, S, MOBA_HEADS, MOBA_HEAD_DIM)
    moba_out = moba_attention(mq, mk, mv)
    mixed = jnp.concatenate([ret_out.astype(h.dtype), rwkv_out.astype(h.dtype),
                             moba_out.astype(h.dtype)], axis=-1)
    return mixed @ w_out


def conv_glu_ffn(h, w_up, conv_w, conv_b, w_down):
    S = h.shape[1]
    u = h @ w_up
    up = jnp.pad(u, ((0, 0), (CONV_WIDTH - 1, 0), (0, 0)))
    u = sum(conv_w[j] * up[:, j:j + S] for j in range(CONV_WIDTH)) + conv_b
    val, gate = jnp.split(u, 2, axis=-1)
    return (jax.nn.silu(gate) * val) @ w_down


def setup_inputs(seed: int = 0) -> dict:
    key = jax.random.key(seed)
    ks = jax.random.split(key, 24)
    D, L = D_MODEL, DEPTH
    f32 = jnp.float32

    def nrm(k, shape, scale):
        return jax.random.normal(k, shape, f32) * scale

    ratio = (jnp.arange(RWKV_WIDTH, dtype=f32) / (RWKV_WIDTH - 1)) ** 0.85
    return {
        "x": nrm(ks[0], (BATCH, SEQ, D), 1.0),
        "c": nrm(ks[1], (BATCH, D), 1.0),
        "w_in": nrm(ks[2], (L, D, IN_COLS), D ** -0.5),
        "w_out": nrm(ks[3], (L, MIX_WIDTH, D), MIX_WIDTH ** -0.5),
        "rwkv_mu": jax.random.uniform(ks[4], (L, RWKV_COLS), f32),
        "rwkv_w0": (-6.5 + 5.0 * ratio)[None, :] + nrm(ks[5], (L, RWKV_WIDTH), 0.1),
        "rwkv_w_up": nrm(ks[6], (L, RWKV_DECAY_RANK, RWKV_WIDTH), 0.1 * RWKV_DECAY_RANK ** -0.5),
        "rwkv_a0": nrm(ks[7], (L, RWKV_WIDTH), 0.1),
        "rwkv_a_up": nrm(ks[8], (L, RWKV_A_RANK, RWKV_WIDTH), RWKV_A_RANK ** -0.5),
        "rwkv_g_up": nrm(ks[9], (L, RWKV_GATE_RANK, RWKV_WIDTH), RWKV_GATE_RANK ** -0.5),
        "rwkv_k_k": 0.85 + nrm(ks[10], (L, RWKV_WIDTH), 0.05),
        "rwkv_k_a": 1.0 + nrm(ks[11], (L, RWKV_WIDTH), 0.05),
        "rwkv_r_k": -0.04 + nrm(ks[12], (L, RWKV_HEADS, RWKV_HEAD_DIM), 0.1),
        "rwkv_ln_w": 1.0 + nrm(ks[13], (L, RWKV_WIDTH), 0.05),
        "rwkv_ln_b": nrm(ks[14], (L, RWKV_WIDTH), 0.02),
        "w_ffn_up": nrm(ks[15], (L, D, 2 * D_FF), D ** -0.5),
        "ffn_conv_w": nrm(ks[16], (L, CONV_WIDTH, 2 * D_FF), CONV_WIDTH ** -0.5),
        "ffn_conv_b": nrm(ks[17], (L, 2 * D_FF), 0.02),
        "w_ffn_down": nrm(ks[18], (L, D_FF, D), D_FF ** -0.5),
        "w_ada": nrm(ks[19], (L, D, 6 * D), 0.5 * D ** -0.5),
        "b_ada": nrm(ks[20], (L, 6 * D), 0.02),
        "norm_mix": 1.0 + nrm(ks[21], (L, D), 0.05),
        "norm_ffn": 1.0 + nrm(ks[22], (L, D), 0.05),
        "norm_final": 1.0 + nrm(ks[23], (D,), 0.05),
    }


def reference(x, c, w_in, w_out, rwkv_mu, rwkv_w0, rwkv_w_up, rwkv_a0, rwkv_a_up, rwkv_g_up,
              rwkv_k_k, rwkv_k_a, rwkv_r_k, rwkv_ln_w, rwkv_ln_b, w_ffn_up, ffn_conv_w,
              ffn_conv_b, w_ffn_down, w_ada, b_ada, norm_mix, norm_ffn, norm_final):
    c_act = jax.nn.silu(c)
    for l in range(DEPTH):
        mod = (c_act @ w_ada[l] + b_ada[l])[:, None, :]
        sh1, sc1, g1, sh2, sc2, g2 = jnp.split(mod, 6, axis=-1)
        h = rms_norm(x, norm_mix[l]) * (1.0 + sc1) + sh1
        x = x + g1 * hybrid_mixer(h, w_in[l], w_out[l], rwkv_mu[l], rwkv_w0[l], rwkv_w_up[l],
                                  rwkv_a0[l], rwkv_a_up[l], rwkv_g_up[l], rwkv_k_k[l],
                                  rwkv_k_a[l], rwkv_r_k[l], rwkv_ln_w[l], rwkv_ln_b[l])
        h = rms_norm(x, norm_ffn[l]) * (1.0 + sc2) + sh2
        x = x + g2 * conv_glu_ffn(h, w_ffn_up[l], ffn_conv_w[l], ffn_conv_b[l], w_ffn_down[l])
    return rms_norm(x, norm_final)
```

```python
import math
from contextlib import ExitStack
import numpy as np
import ml_dtypes
import concourse.bass as bass
import concourse.mybir as mybir
from concourse.bass_utils import run_bass_kernel_spmd

F32 = mybir.dt.float32
BF16 = mybir.dt.bfloat16
AF = mybir.ActivationFunctionType
ALU = mybir.AluOpType
AX = mybir.AxisListType
NPBF = ml_dtypes.bfloat16

D = 2048
S = 4096
B = 2
NT = 1024
NH = 2
NTH = NT + NH
KC = 16
DFF = 5632
CP = 44
EPS = 1e-6
GN_EPS = 64e-5
TILES = [(0, 512), (512, 1024)]
TILES_H = [(0, 2), (2, 514), (514, 1026)]

COMPUTE = ("pe", "act", "dve", "pool")
NS_POOL = {"sp": 8, "act": 4, "pool": 8}
_CNT = [0]
_DBG = {}


class Op:
    __slots__ = ("id", "stream", "is_dma", "fn", "deps", "waits", "signaled", "count",
                 "dma_idx", "sem", "val", "idx_in_stream")


class Prog:
    def __init__(self, nc):
        self.nc = nc
        self.stack = ExitStack()
        self.ops = []
        self.state = {}
        self.dma_count = {"sp": 0, "act": 0, "pool": 0}
        self.dma_ops = {"sp": [], "act": [], "pool": []}

    def sb(self, shape, dtype, name=None):
        _CNT[0] += 1
        return self.stack.enter_context(self.nc.sbuf_tensor((name or "t") + f"_{_CNT[0]}", list(shape), dtype))

    def ps(self, shape, dtype=F32, name=None):
        _CNT[0] += 1
        return self.stack.enter_context(self.nc.psum_tensor((name or "p") + f"_{_CNT[0]}", list(shape), dtype))

    def banks(self, n=8):
        return [self.ps([128, 512], F32, name=f"bank{i}") for i in range(n)]

    def _add(self, stream, is_dma, fn, reads, writes):
        op = Op()
        op.id = len(self.ops)
        op.stream = stream
        op.is_dma = is_dma
        op.fn = fn
        deps = set()
        r2, w2 = [], []
        for k in writes:
            w2.append(("bank", k[1]) if isinstance(k, tuple) and k and k[0] == "s" else k)
        for k in reads:
            if isinstance(k, tuple) and k and k[0] == "s":
                k = ("bank", k[1])
            if isinstance(k, tuple) and k and k[0] == "bank":
                w2.append(k)
            else:
                r2.append(k)
        reads, writes = r2, w2
        for k in reads:
            st = self.state.get(k)
            if st is None:
                st = self.state[k] = [None, []]
            if st[0] is not None:
                deps.add(st[0])
            st[1].append(op.id)
        for k in writes:
            st = self.state.get(k)
            if st is None:
                st = self.state[k] = [None, []]
            if st[0] is not None:
                deps.add(st[0])
            for r in st[1]:
                if r != op.id:
                    deps.add(r)
            st[0] = op.id
            st[1] = []
        if is_dma:
            q = stream
            op.dma_idx = self.dma_count[q]
            self.dma_count[q] += 1
            ns = NS_POOL[q]
            if op.dma_idx >= ns:
                deps.add(self.dma_ops[q][op.dma_idx - ns].id)
            self.dma_ops[q].append(op)
        op.deps = deps
        op.signaled = is_dma
        self.ops.append(op)
        return op

    def pe(self, fn, reads=(), writes=()):
        return self._add("pe", False, fn, reads, writes)

    def act(self, fn, reads=(), writes=()):
        return self._add("act", False, fn, reads, writes)

    def dve(self, fn, reads=(), writes=()):
        return self._add("dve", False, fn, reads, writes)

    def pool(self, fn, reads=(), writes=()):
        return self._add("pool", False, fn, reads, writes)

    def dma(self, out, in_, reads=(), writes=(), q="sp"):
        return self._add(q, True, lambda e: e.dma_start(out=out, in_=in_), reads, writes)

    def finish(self):
        r = self.flush()
        self.stack.close()
        return r

    def flush(self):
        nc = self.nc
        ops = self.ops
        if not ops:
            return {}
        handles = []
        streams = {s: [] for s in ("pe", "act", "dve", "pool", "sp")}
        for op in ops:
            op.idx_in_stream = len(streams[op.stream])
            streams[op.stream].append(op)
        for s, lst in streams.items():
            waited_eng = {e: -1 for e in COMPUTE}
            waited_dma = {}
            for op in lst:
                need_eng = {}
                need_dma = {}
                for d in op.deps:
                    dop = ops[d]
                    if dop.is_dma:
                        q = dop.stream
                        key = (q, dop.dma_idx % NS_POOL[q])
                        if waited_dma.get(key, -1) >= dop.dma_idx:
                            continue
                        if need_dma.get(key, -1) < dop.dma_idx:
                            need_dma[key] = dop.dma_idx
                    else:
                        e = dop.stream
                        if e == "pe" and s == "pe" and not op.is_dma:
                            continue
                        if waited_eng[e] >= dop.idx_in_stream:
                            continue
                        if need_eng.get(e, -1) < dop.idx_in_stream:
                            need_eng[e] = dop.idx_in_stream
                waits = []
                for e, i in need_eng.items():
                    waited_eng[e] = i
                    tgt = streams[e][i]
                    tgt.signaled = True
                    waits.append(("eng", e, tgt))
                for key, i in need_dma.items():
                    waited_dma[key] = i
                    waits.append(("dma", key, self.dma_ops[key[0]][i]))
                op.waits = waits
        last_ops = {}
        for e in COMPUTE:
            for op in reversed(streams[e]):
                if not op.is_dma:
                    op.signaled = True
                    last_ops[e] = op
                    break
        for e in COMPUTE:
            c = 0
            for op in streams[e]:
                if not op.is_dma and op.signaled:
                    c += 1
                    op.count = c
        sems = {}
        for e in COMPUTE:
            _CNT[0] += 1
            sems[e] = nc.alloc_semaphore(name=f"s_{e}_{_CNT[0]}")
            handles.append(sems[e])
        dsem = {}
        for q, ns in NS_POOL.items():
            if not self.dma_ops[q]:
                continue
            for i in range(ns):
                _CNT[0] += 1
                dsem[(q, i)] = nc.alloc_semaphore(name=f"d_{q}{i}_{_CNT[0]}")
                handles.append(dsem[(q, i)])
        for q in NS_POOL:
            ns = NS_POOL[q]
            for op in self.dma_ops[q]:
                op.sem = dsem[(q, op.dma_idx % ns)]
                op.val = 16 * (op.dma_idx // ns + 1)

        def run_stream(eng, lst, final=False):
            for op in lst:
                for w in op.waits:
                    if w[0] == "eng":
                        eng.wait_ge(sems[w[1]], w[2].count)
                    else:
                        eng.wait_ge(w[2].sem, w[2].val)
                inst = op.fn(eng)
                if op.is_dma:
                    inst.then_inc(op.sem, 16)
                elif op.signaled:
                    inst.then_inc(sems[op.stream], 1)
            if final:
                for q in NS_POOL:
                    ns = NS_POOL[q]
                    lastv = {}
                    for op in self.dma_ops[q]:
                        lastv[op.dma_idx % ns] = op
                    for op in lastv.values():
                        eng.wait_ge(op.sem, op.val)
                for e_, op in last_ops.items():
                    eng.wait_ge(sems[e_], op.count)

        with nc.Block() as block:
            @block.tensor
            def _(e):
                run_stream(e, streams["pe"])

            @block.scalar
            def _(e):
                run_stream(e, streams["act"])

            @block.vector
            def _(e):
                run_stream(e, streams["dve"])

            @block.gpsimd
            def _(e):
                run_stream(e, streams["pool"])

            @block.sync
            def _(e):
                run_stream(e, streams["sp"], final=True)
        nc.all_engine_barrier()
        nc.clear_and_free_semaphores(handles)
        nc.all_engine_barrier()
        self.ops = []
        self.state = {}
        self.dma_count = {"sp": 0, "act": 0, "pool": 0}
        self.dma_ops = {"sp": [], "act": [], "pool": []}
        return {s: len(l) for s, l in streams.items()}


class IO:
    def __init__(self, nc):
        self.nc = nc
        self.ins = []
        self.outs = []

    def inp(self, name, shape, dtype):
        self.ins.append(name)
        return self.nc.dram_tensor(name, list(shape), dtype, kind="ExternalInput").ap()

    def out(self, name, shape, dtype):
        self.outs.append(name)
        return self.nc.dram_tensor(name, list(shape), dtype, kind="ExternalOutput").ap()

    def scr(self, name, shape, dtype):
        return self.nc.dram_tensor(name, list(shape), dtype, kind="Internal").ap()


def fm(ap):
    return ap.rearrange("(k p) t -> p k t", p=128)


def phase_mod(nc, cP, wada, bada, modS):
    P = Prog(nc)
    cact = P.sb([128, 2, 16], F32)
    P.dma(cact[:], cP, writes=["cact"])
    P.act(lambda e: e.activation(cact[:], cact[:], AF.Silu), reads=["cact"], writes=["cact"])
    bsb = P.sb([128, 2, 12], F32)
    P.dma(bsb[:], bada, writes=["bsb"])
    ps = P.ps([128, 2, 12, 2], F32)
    wbuf = [P.sb([128, 16, 512], F32) for _ in range(2)]
    res = P.sb([128, 2, 2, 12], F32)
    i = 0
    for l in range(2):
        for ct in range(3):
            wb = wbuf[i % 2]
            key = ("w", i % 2)
            P.dma(wb[:], wada[l, :, :, ct * 512:(ct + 1) * 512], writes=[key])
            for j in range(4):
                cc = ct * 4 + j
                for kc in range(16):
                    P.pe(lambda e, wb=wb, j=j, kc=kc, l=l, cc=cc: e.matmul(
                        ps[:, l, cc, :], wb[:, kc, j * 128:(j + 1) * 128], cact[:, :, kc],
                        start=(kc == 0), stop=(kc == 15)), reads=[key, "cact"], writes=["ps"])
            i += 1
    for l in range(2):
        for b in range(2):
            P.dve(lambda e, l=l, b=b: e.tensor_tensor(res[:, l, b, :], ps[:, l, :, b], bsb[:, l, :], ALU.add),
                  reads=["ps", "bsb"], writes=["res"])
    P.dma(modS, res[:], reads=["res"])
    return P.finish()


def phase_norm(nc, x_dram, N, tiles, normP_dram, h_dram, out_dt, modP_dram=None, sc_off=0, sh_off=0,
               flag_dram=None, nhalo=0):
    P = Prog(nc)
    xT = P.sb([128, KC, N], F32)
    for q4 in range(4):
        P.dma(xT[:, q4 * 4:(q4 + 1) * 4, :], fm(x_dram)[:, q4 * 4:(q4 + 1) * 4, :], writes=[("x", q4)])
    nrm = P.sb([128, 16], F32)
    P.dma(nrm[:], normP_dram, writes=["nrm"])
    gam = P.sb([128, 16], F32)
    if modP_dram is not None:
        modP = P.sb([128, 96], F32)
        P.dma(modP[:], modP_dram, writes=["modP"])
        P.dve(lambda e: e.scalar_tensor_tensor(gam[:], modP[:, sc_off:sc_off + 16], 1.0, nrm[:], ALU.add, ALU.mult),
              reads=["modP", "nrm"], writes=["gam"])
    else:
        P.dve(lambda e: e.tensor_copy(gam[:], nrm[:]), reads=["nrm"], writes=["gam"])
    if flag_dram is not None:
        flag = P.sb([128, 1], F32)
        P.dma(flag[:], flag_dram, writes=["flag"])
    ones = P.sb([128, 128], F32)
    P.pool(lambda e: e.memset(ones[:], 1.0), writes=["ones"])
    epsT = P.sb([128, 1], F32)
    P.pool(lambda e: e.memset(epsT[:], EPS), writes=["eps"])
    sq = [P.sb([128, N], F32) for _ in range(2)]
    pss = [P.ps([128, 512], F32) for _ in tiles]
    for kc in range(KC):
        s_ = sq[kc % 2]
        P.act(lambda e, s_=s_, kc=kc: e.activation(s_[:], xT[:, kc, :], AF.Square),
              reads=[("x", kc // 4)], writes=[("sq", kc % 2)])
        for ti, (a, b) in enumerate(tiles):
            P.pe(lambda e, s_=s_, ti=ti, a=a, b=b, kc=kc: e.matmul(
                pss[ti][:, 0:b - a], ones[:], s_[:, a:b], start=(kc == 0), stop=(kc == KC - 1)),
                reads=[("sq", kc % 2), "ones"], writes=[("pss", ti)])
    rstd = P.sb([128, N], F32)
    for ti, (a, b) in enumerate(tiles):
        P.act(lambda e, ti=ti, a=a, b=b: e.activation(rstd[:, a:b], pss[ti][:, 0:b - a], AF.Sqrt,
                                                      bias=epsT[:], scale=1.0 / D),
              reads=[("pss", ti), "eps"], writes=["rstd"])
    P.dve(lambda e: e.reciprocal(rstd[:], rstd[:]), reads=["rstd"], writes=["rstd"])
    tmp = [P.sb([128, N], F32) for _ in range(2)]
    hb = [P.sb([128, N], out_dt) for _ in range(2)]
    for kc in range(KC):
        t_ = tmp[kc % 2]
        h_ = hb[kc % 2]
        P.dve(lambda e, t_=t_, kc=kc: e.tensor_tensor(t_[:], xT[:, kc, :], rstd[:], ALU.mult),
              reads=[("x", kc // 4), "rstd"], writes=[("tmp", kc % 2)])
        if modP_dram is not None:
            P.act(lambda e, t_=t_, h_=h_, kc=kc: e.activation(
                h_[:], t_[:], AF.Identity, bias=modP[:, sh_off + kc:sh_off + kc + 1], scale=gam[:, kc:kc + 1]),
                reads=[("tmp", kc % 2), "gam", "modP"], writes=[("hb", kc % 2)])
        else:
            P.act(lambda e, t_=t_, h_=h_, kc=kc: e.activation(
                h_[:], t_[:], AF.Copy, scale=gam[:, kc:kc + 1]),
                reads=[("tmp", kc % 2), "gam"], writes=[("hb", kc % 2)])
        if flag_dram is not None:
            P.dve(lambda e, h_=h_: e.tensor_scalar(h_[:, 0:nhalo], h_[:, 0:nhalo], flag[:, 0:1], None, ALU.mult),
                  reads=[("hb", kc % 2), "flag"], writes=[("hb", kc % 2)])
        P.dma(h_dram[kc * 128:(kc + 1) * 128, :], h_[:], reads=[("hb", kc % 2)], q="sp")
    return P.finish()


def phase_wout(nc, x_dram, mixT_dram, wout_dram, modP_dram, xmid_dram):
    P = Prog(nc)
    W = P.sb([128, KC, D], BF16)
    for q4 in range(4):
        P.dma(W[:, q4 * 4:(q4 + 1) * 4, :], wout_dram[:, q4 * 4:(q4 + 1) * 4, :], writes=[("W", q4)], q="pool")
    mixT = P.sb([128, KC, NTH], BF16)
    P.dma(mixT[:], fm(mixT_dram), writes=["mix"])
    xT = P.sb([128, KC, NTH], F32)
    for q4 in range(4):
        P.dma(xT[:, q4 * 4:(q4 + 1) * 4, :], fm(x_dram)[:, q4 * 4:(q4 + 1) * 4, :], writes=[("x", q4 * 4 + j) for j in range(4)])
    modP = P.sb([128, 96], F32)
    P.dma(modP[:], modP_dram, writes=["modP"])
    banks = P.banks(4)
    i = 0
    for dc in range(KC):
        for (a, b) in TILES_H:
            ps = banks[i % 4]
            pk = ("ps", i % 4)
            for kc in range(KC):
                P.pe(lambda e, ps=ps, kc=kc, dc=dc, a=a, b=b: e.matmul(
                    ps[:, 0:b - a], W[:, kc, dc * 128:(dc + 1) * 128], mixT[:, kc, a:b],
                    start=(kc == 0), stop=(kc == KC - 1)), reads=[("W", kc // 4), "mix"], writes=[pk])
            P.dve(lambda e, ps=ps, dc=dc, a=a, b=b: e.scalar_tensor_tensor(
                xT[:, dc, a:b], ps[:, 0:b - a], modP[:, 32 + dc:33 + dc], xT[:, dc, a:b], ALU.mult, ALU.add),
                reads=[pk, "modP", ("x", dc)], writes=[("x", dc)])
            i += 1
        P.dma(xmid_dram[dc * 128:(dc + 1) * 128, :], xT[:, dc, :], reads=[("x", dc)], q="sp")
    return P.finish()


def phase_ffn_up(nc, h2_dram, wup_dram, convP_dram, aT):
    P = Prog(nc)
    h2T = P.sb([128, KC, NTH], BF16)
    P.dma(h2T[:], fm(h2_dram), writes=["h2"])
    convP = P.sb([128, CP, 8], F32)
    P.dma(convP[:], convP_dram, writes=["convP"])
    wbuf = [P.sb([128, 2, KC, 128], BF16) for _ in range(2)]
    U = [[P.sb([128, NTH], F32) for _ in range(2)] for _ in range(2)]
    cvb = [[P.sb([128, NT], F32) for _ in range(2)] for _ in range(2)]
    sgb = [P.sb([128, NT], F32) for _ in range(2)]
    banks = P.banks(6)
    i = 0
    for cp in range(CP):
        par = cp % 2
        wb = wbuf[par]
        P.dma(wb[:], wup_dram[cp], writes=[("wb", par)], q="pool")
        for half in range(2):
            Ub = U[half][par]
            for (a, b) in TILES_H:
                ps = banks[i % 6]
                pk = ("ps", i % 6)
                for kc in range(KC):
                    P.pe(lambda e, ps=ps, wb=wb, half=half, kc=kc, a=a, b=b: e.matmul(
                        ps[:, 0:b - a], wb[:, half, kc, :], h2T[:, kc, a:b],
                        start=(kc == 0), stop=(kc == KC - 1)), reads=[("wb", par), "h2"], writes=[pk])
                P.act(lambda e, ps=ps, Ub=Ub, a=a, b=b: e.copy(Ub[:, a:b], ps[:, 0:b - a]),
                      reads=[pk], writes=[("U", half, par)])
                i += 1
        for half in range(2):
            Ub = U[half][par]
            cv = cvb[half][par]
            o = 4 * half
            P.act(lambda e, Ub=Ub, cv=cv, cp=cp, o=o: e.activation(
                cv[:], Ub[:, 2:NTH], AF.Identity, bias=convP[:, cp, o + 3:o + 4], scale=convP[:, cp, o + 2:o + 3]),
                reads=[("U", half, par), "convP"], writes=[("cv", half, par)])
            P.dve(lambda e, Ub=Ub, cv=cv, cp=cp, o=o: e.scalar_tensor_tensor(
                cv[:], Ub[:, 1:NTH - 1], convP[:, cp, o + 1:o + 2], cv[:], ALU.mult, ALU.add),
                reads=[("U", half, par), "convP", ("cv", half, par)], writes=[("cv", half, par)])
            P.dve(lambda e, Ub=Ub, cv=cv, cp=cp, o=o: e.scalar_tensor_tensor(
                cv[:], Ub[:, 0:NT], convP[:, cp, o:o + 1], cv[:], ALU.mult, ALU.add),
                reads=[("U", half, par), "convP", ("cv", half, par)], writes=[("cv", half, par)])
        sg = sgb[par]
        P.act(lambda e, sg=sg, cg=cvb[1][par]: e.activation(sg[:], cg[:], AF.Silu),
              reads=[("cv", 1, par)], writes=[("sg", par)])
        P.pool(lambda e, sg=sg, cv=cvb[0][par], cp=cp: e.tensor_tensor(aT[:, cp, :], cv[:], sg[:], ALU.mult),
               reads=[("cv", 0, par), ("sg", par)], writes=[("aT", cp)])
    return P.finish()


def phase_ffn_down(nc, aT, wdn_dram, xmid_dram, modP_dram, xout_dram):
    P = Prog(nc)
    modP = P.sb([128, 96], F32)
    P.dma(modP[:], modP_dram, writes=["modP"])
    wbuf = [P.sb([128, CP, 128], BF16) for _ in range(2)]
    xm = [P.sb([128, NT], F32) for _ in range(2)]
    xo = [P.sb([128, NT], F32) for _ in range(2)]
    banks = P.banks(4)
    i = 0
    for dc in range(KC):
        par = dc % 2
        wb = wbuf[par]
        P.dma(wb[:], wdn_dram[dc], writes=[("wb", par)], q="pool")
        P.dma(xm[par][:], xmid_dram[dc * 128:(dc + 1) * 128, NH:NTH], writes=[("xm", par)], q="sp")
        for (a, b) in TILES:
            ps = banks[i % 4]
            pk = ("ps", i % 4)
            for cc in range(CP):
                P.pe(lambda e, ps=ps, wb=wb, cc=cc, a=a, b=b: e.matmul(
                    ps[:, 0:b - a], wb[:, cc, :], aT[:, cc, a:b], start=(cc == 0), stop=(cc == CP - 1)),
                    reads=[("wb", par)], writes=[pk])
            P.dve(lambda e, ps=ps, par=par, dc=dc, a=a, b=b: e.scalar_tensor_tensor(
                xo[par][:, a:b], ps[:, 0:b - a], modP[:, 80 + dc:81 + dc], xm[par][:, a:b], ALU.mult, ALU.add),
                reads=[pk, "modP", ("xm", par)], writes=[("xo", par)])
            i += 1
        P.dma(xout_dram[dc * 128:(dc + 1) * 128, :], xo[par][:], reads=[("xo", par)], q="sp")
    return P.finish()


FCH = [("rq", 64, None, BF16), ("rk", 64, None, BF16), ("rg", 128, None, F32),
       ("wr", 128, 0, F32), ("wk", 128, 1, F32), ("wv", 128, 2, F32), ("wd", 96, 3, F32), ("wa", 96, 4, F32),
       ("wg0", 128, 5, F32), ("wg1", 128, 6, F32),
       ("mq0", 128, None, BF16), ("mq1", 128, None, BF16), ("mk0", 128, None, BF16), ("mk1", 128, None, BF16)]
NF = sum(c[1] for c in FCH)
NTK = 448


def phase_inproj(nc, hT_dram, wf_dram, wt_dram, muP_dram, fo, to):
    P = Prog(nc)
    Wf = P.sb([128, KC, NF], BF16)
    Wt = P.sb([128, KC, NTK], BF16)
    for q4 in range(4):
        P.dma(Wf[:, q4 * 4:(q4 + 1) * 4, :], wf_dram[:, q4 * 4:(q4 + 1) * 4, :], writes=[("Wf", q4)], q="pool")
        P.dma(Wt[:, q4 * 4:(q4 + 1) * 4, :], wt_dram[:, q4 * 4:(q4 + 1) * 4, :], writes=[("Wt", q4)], q="pool")
    mu = P.sb([128, 7], F32)
    P.dma(mu[:], muP_dram, writes=["mu"])
    hbuf = [P.sb([128, KC, 512], BF16) for _ in range(2)]
    rwbuf = [P.sb([128, 513], F32) for _ in range(7)]
    for r in range(7):
        P.pool(lambda e, r=r: e.memset(rwbuf[r][:], 0.0), writes=[("rwb", r)])
    tmpb = [P.sb([128, 512], F32) for _ in range(2)]
    o32 = [P.sb([128, 512], F32) for _ in range(3)]
    o16 = [P.sb([128, 512], BF16) for _ in range(3)]
    t16 = [P.sb([128, NTK], BF16) for _ in range(2)]
    banks = P.banks(8)
    i32 = 0
    i16 = 0
    ib = 0
    it = 0
    for T in range(8):
        hb = hbuf[T % 2]
        hk = ("h", T % 2)
        P.dma(hb[:], fm(hT_dram)[:, :, T * 512:(T + 1) * 512], writes=[hk])
        off = 0
        for (name, ncol, r, dt) in FCH:
            ps = banks[ib % 5]
            pk = ("ps", ib % 5)
            ib += 1
            for kc in range(KC):
                P.pe(lambda e, ps=ps, kc=kc, off=off, ncol=ncol, hb=hb: e.matmul(
                    ps[0:ncol, :], Wf[:, kc, off:off + ncol], hb[:, kc, :], start=(kc == 0), stop=(kc == KC - 1)),
                    reads=[("Wf", kc // 4), hk], writes=[pk])
            if r is not None:
                buf = rwbuf[r]
                bk = ("rwb", r)
                if T > 0:
                    P.dve(lambda e, buf=buf, ncol=ncol: e.tensor_copy(buf[0:ncol, 0:1], buf[0:ncol, 512:513]),
                          reads=[bk], writes=[bk])
                P.act(lambda e, buf=buf, ps=ps, ncol=ncol: e.copy(buf[0:ncol, 1:513], ps[0:ncol, :]),
                      reads=[pk], writes=[bk])
                tb = tmpb[i32 % 2]
                tk = ("tmp", i32 % 2)
                ob = o32[i32 % 3]
                ok = ("o32", i32 % 3)
                i32 += 1
                P.dve(lambda e, buf=buf, tb=tb, ncol=ncol: e.tensor_tensor(
                    tb[0:ncol, :], buf[0:ncol, 0:512], buf[0:ncol, 1:513], ALU.subtract), reads=[bk], writes=[tk])
                P.dve(lambda e, buf=buf, tb=tb, ob=ob, ncol=ncol, r=r: e.scalar_tensor_tensor(
                    ob[0:ncol, :], tb[0:ncol, :], mu[0:ncol, r:r + 1], buf[0:ncol, 1:513], ALU.mult, ALU.add),
                    reads=[bk, tk, "mu"], writes=[ok])
                P.dma(fo[name][:, T * 512:(T + 1) * 512], ob[0:ncol, :], reads=[ok], q="sp")
            elif dt == F32:
                ob = o32[i32 % 3]
                ok = ("o32", i32 % 3)
                i32 += 1
                P.act(lambda e, ob=ob, ps=ps, ncol=ncol: e.copy(ob[0:ncol, :], ps[0:ncol, :]), reads=[pk], writes=[ok])
                P.dma(fo[name][:, T * 512:(T + 1) * 512], ob[0:ncol, :], reads=[ok], q="sp")
            else:
                ob = o16[i16 % 3]
                ok = ("o16", i16 % 3)
                i16 += 1
                P.dve(lambda e, ob=ob, ps=ps, ncol=ncol: e.tensor_copy(ob[0:ncol, :], ps[0:ncol, :]), reads=[pk], writes=[ok])
                P.dma(fo[name][:, T * 512:(T + 1) * 512], ob[0:ncol, :], reads=[ok], q="sp")
            off += ncol
        for s4 in range(4):
            ps = banks[5 + it % 3]
            pk = ("pst", it % 3)
            tb = t16[it % 2]
            tk = ("t16", it % 2)
            it += 1
            for kc in range(KC):
                P.pe(lambda e, ps=ps, kc=kc, hb=hb, s4=s4: e.matmul(
                    ps[:, 0:NTK], hb[:, kc, s4 * 128:(s4 + 1) * 128], Wt[:, kc, :], start=(kc == 0), stop=(kc == KC - 1)),
                    reads=[("Wt", kc // 4), hk], writes=[pk])
            P.act(lambda e, ps=ps, tb=tb: e.copy(tb[:], ps[:, 0:NTK]), reads=[pk], writes=[tk])
            t0 = T * 512 + s4 * 128
            P.dma(to["rv"][t0:t0 + 128, :], tb[:, 0:128], reads=[tk], q="sp")
            P.dma(to["rkt"][t0:t0 + 128, :], tb[:, 128:192], reads=[tk], q="sp")
            P.dma(to["mv"][t0:t0 + 128, :], tb[:, 192:448], reads=[tk], q="sp")
    return P.finish()


def phase_ret(nc, rq, rk, rg, rv, rkt, cst, out_dram):
    P = Prog(nc)
    qT = P.sb([64, S], BF16)
    kT = P.sb([64, S], BF16)
    gT = P.sb([128, S], F32)
    v = P.sb([128, 32, 128], BF16)
    kt = P.sb([128, 32, 64], BF16)
    P.dma(qT[:], rq, writes=["qT"])
    P.dma(kT[:], rk, writes=["kT"])
    P.dma(gT[:], rg, writes=["gT"])
    P.dma(v[:], rv.rearrange("(n p) d -> p n d", p=128), writes=["v"])
    P.dma(kt[:], rkt.rearrange("(n p) d -> p n d", p=128), writes=["kt"])
    decT = P.sb([128, 128], F32)
    qdec = P.sb([64, 128], F32)
    kdec = P.sb([128, 1], F32)
    gC = P.sb([64, 1], F32)
    P.dma(decT[:], cst["decT"], writes=["decT"])
    P.dma(qdec[:], cst["qdec"], writes=["qdec"])
    P.dma(kdec[:], cst["kdec"], writes=["kdec"])
    P.dma(gC[:], cst["gC"], writes=["gC"])
    ones = P.sb([128, 128], F32)
    P.pool(lambda e: e.memset(ones[:], 1.0), writes=["ones"])
    epsT = P.sb([128, 1], F32)
    P.pool(lambda e: e.memset(epsT[:], EPS), writes=["eps"])
    qd = P.sb([64, 32, 128], BF16)
    P.dve(lambda e: e.tensor_tensor(qd[:], qT[:].rearrange("p (n c) -> p n c", c=128),
                                    qdec[:].unsqueeze(1).broadcast_to([64, 32, 128]), ALU.mult),
          reads=["qT", "qdec"], writes=["qd"])
    kd = P.sb([128, 32, 64], BF16)
    P.dve(lambda e: e.tensor_scalar(kd[:], kt[:], kdec[:, 0:1], None, ALU.mult), reads=["kt", "kdec"], writes=["kd"])
    sg = P.sb([128, S], F32)
    for q8 in range(8):
        P.act(lambda e, q8=q8: e.activation(sg[:, q8 * 512:(q8 + 1) * 512], gT[:, q8 * 512:(q8 + 1) * 512], AF.Silu),
              reads=["gT"], writes=[("sg", q8)])
    R32 = P.sb([64, 128], F32)
    Rb = P.sb([64, 128], BF16)
    P.pool(lambda e: e.memset(R32[:], 0.0), writes=["R32"])
    P.pool(lambda e: e.memset(Rb[:], 0.0), writes=["Rb"])
    Ab = [P.sb([128, 128], BF16) for _ in range(2)]
    sq = [P.sb([128, 128], F32) for _ in range(2)]
    rs = [P.sb([128, 128], F32) for _ in range(2)]
    ob = [P.sb([128, 128], F32) for _ in range(2)]
    outT = P.sb([128, S], BF16)
    banks = P.banks(8)
    for n in range(32):
        par = n % 2
        cs = slice(n * 128, (n + 1) * 128)
        psA = banks[par][:, 0:128]
        psO = banks[2 + par][:, 0:128]
        psR = banks[4 + par][0:64, 0:128]
        psS = banks[6 + par][:, 0:128]
        P.pe(lambda e, psA=psA, cs=cs: e.matmul(psA, kT[:, cs], qT[:, cs], start=True, stop=True),
             reads=["kT", "qT"], writes=[("psA", par)])
        P.dve(lambda e, psA=psA, par=par: e.tensor_tensor(Ab[par][:], psA, decT[:], ALU.mult),
              reads=[("psA", par), "decT"], writes=[("Ab", par)])
        P.pe(lambda e, psO=psO, n=n, par=par: e.matmul(psO, v[:, n, :], Ab[par][:], start=True, stop=False),
             reads=["v", ("Ab", par)], writes=[("psO", par)])
        P.pe(lambda e, psO=psO, n=n: e.matmul(psO, Rb[:], qd[:, n, :], start=False, stop=True),
             reads=["Rb", "qd"], writes=[("psO", par)])
        P.pe(lambda e, psR=psR, n=n: e.matmul(psR, kd[:, n, :], v[:, n, :], start=True, stop=True),
             reads=["kd", "v"], writes=[("psR", par)])
        P.dve(lambda e, psR=psR: e.scalar_tensor_tensor(R32[:], R32[:], gC[:, 0:1], psR, ALU.mult, ALU.add),
              reads=[("psR", par), "gC", "R32"], writes=["R32"])
        P.act(lambda e: e.copy(Rb[:], R32[:]), reads=["R32"], writes=["Rb"])
        P.act(lambda e, psO=psO, par=par: e.activation(sq[par][:], psO, AF.Square),
              reads=[("psO", par)], writes=[("sq", par)])
        P.pe(lambda e, psS=psS, par=par: e.matmul(psS, ones[:], sq[par][:], start=True, stop=True),
             reads=["ones", ("sq", par)], writes=[("psS", par)])
        P.act(lambda e, psS=psS, par=par: e.activation(rs[par][:], psS, AF.Sqrt, bias=epsT[:], scale=1.0 / 128),
              reads=[("psS", par), "eps"], writes=[("rs", par)])
        P.dve(lambda e, par=par: e.reciprocal(rs[par][:], rs[par][:]), reads=[("rs", par)], writes=[("rs", par)])
        P.dve(lambda e, psO=psO, par=par: e.tensor_tensor(ob[par][:], psO, rs[par][:], ALU.mult),
              reads=[("psO", par), ("rs", par)], writes=[("ob", par)])
        P.pool(lambda e, par=par, cs=cs: e.tensor_tensor(outT[:, cs], ob[par][:], sg[:, cs], ALU.mult),
               reads=[("ob", par), ("sg", n // 4)], writes=[("outT", n // 8)])
        if n % 8 == 7:
            q8 = n // 8
            P.dma(out_dram[:, q8 * 1024:(q8 + 1) * 1024], outT[:, q8 * 1024:(q8 + 1) * 1024], reads=[("outT", q8)], q="sp")
    return P.finish()


def phase_moba(nc, mq, mk, mv, hh, cst, out_dram):
    P = Prog(nc)
    qT = P.sb([128, S], BF16)
    kT = P.sb([128, S], BF16)
    V1 = P.sb([128, 32, 129], BF16)
    P.dma(qT[:], mq, writes=["qT"])
    P.dma(kT[:], mk, writes=["kT"])
    P.dma(V1[:, :, 0:128], mv.rearrange("(n p) d -> p n d", p=128)[:, :, hh * 128:(hh + 1) * 128], writes=["V1"])
    P.pool(lambda e: e.memset(V1[:, :, 128:129], 1.0), writes=["V1"])
    biasS = P.sb([128, 1], F32)
    Ew = P.sb([128, 32, 32], F32)
    tri = P.sb([128, 128], BF16)
    negm = P.sb([128, 32, 16], F32)
    ownm = P.sb([128, 32, 16], F32)
    identb = P.sb([128, 128], BF16)
    P.dma(biasS[:], cst["biasS"][hh], writes=["biasS"])
    P.dma(Ew[:], cst["Ew"][hh], writes=["Ew"])
    P.dma(tri[:], cst["tri"], writes=["tri"], q="pool")
    P.dma(negm[:], cst["negm"], writes=["negm"])
    P.dma(ownm[:], cst["ownm"], writes=["ownm"])
    P.dma(identb[:], cst["ident"], writes=["identb"], q="pool")
    banks = P.banks(8)
    km = P.sb([128, 16], F32)
    P.dve(lambda e: e.tensor_reduce(km[:], kT[:].rearrange("p (n l) -> p n l", l=256), AX.X, ALU.add),
          reads=["kT"], writes=["km"])
    P.dve(lambda e: e.tensor_scalar(km[:], km[:], 1.0 / (256.0 * math.sqrt(128.0)), None, ALU.mult),
          reads=["km"], writes=["km"])
    kmh = P.sb([128, 16], BF16)
    kml = P.sb([128, 16], BF16)
    kmr = P.sb([128, 16], F32)
    P.dve(lambda e: e.tensor_copy(kmh[:], km[:]), reads=["km"], writes=["kmh"])
    P.dve(lambda e: e.tensor_copy(kmr[:], kmh[:]), reads=["kmh"], writes=["kmr"])
    P.dve(lambda e: e.tensor_tensor(kmr[:], km[:], kmr[:], ALU.subtract), reads=["km", "kmr"], writes=["kmr"])
    P.dve(lambda e: e.tensor_copy(kml[:], kmr[:]), reads=["kmr"], writes=["kml"])
    psG = banks[7]
    for qi in range(32):
        P.pe(lambda e, qi=qi: e.matmul(psG[:, qi * 16:(qi + 1) * 16], qT[:, qi * 128:(qi + 1) * 128], kmh[:],
                                       start=True, stop=False), reads=["qT", "kmh"], writes=["psG"])
        P.pe(lambda e, qi=qi: e.matmul(psG[:, qi * 16:(qi + 1) * 16], qT[:, qi * 128:(qi + 1) * 128], kml[:],
                                       start=False, stop=True), reads=["qT", "kml"], writes=["psG"])
    gm = P.sb([128, 32, 16], F32)
    P.dve(lambda e: e.tensor_tensor(gm[:].rearrange("p a b -> p (a b)"), psG[:], negm[:].rearrange("p a b -> p (a b)"),
                                    ALU.add), reads=["psG", "negm"], writes=["gm"])
    sel = P.sb([128, 32, 16], F32)
    P.pool(lambda e: e.memset(sel[:], 0.0), writes=["sel"])
    top8 = [P.sb([128, 8], F32) for _ in range(2)]
    for qi in range(2, 32):
        t8 = top8[qi % 2]
        P.dve(lambda e, t8=t8, qi=qi: e.max(t8[:], gm[:, qi, :]), reads=["gm"], writes=[("t8", qi % 2)])
        P.dve(lambda e, t8=t8, qi=qi: e.tensor_scalar(sel[:, qi, :], gm[:, qi, :], t8[:, 2:3], None, ALU.is_ge),
              reads=["gm", ("t8", qi % 2), "sel"], writes=["sel"])
    P.dve(lambda e: e.tensor_tensor(sel[:], sel[:], ownm[:], ALU.max), reads=["sel", "ownm"], writes=["sel"])
    w = P.sb([128, 32, 32], F32)
    P.dve(lambda e: e.tensor_tensor(w[:].rearrange("p q (n t) -> p q n t", t=2),
                                    Ew[:].rearrange("p q (n t) -> p q n t", t=2),
                                    sel[:].unsqueeze(3).broadcast_to([128, 32, 16, 2]), ALU.mult),
          reads=["Ew", "sel"], writes=["w"])
    acc = P.sb([128, 32, 129], F32)
    PT = [P.sb([128, 512], BF16) for _ in range(2)]
    scale = 1.0 / math.sqrt(128.0)
    i = 0
    j = 0
    for T in range(8):
        for kc in range(4 * T + 4):
            par = i % 2
            i += 1
            psS = banks[par]
            P.pe(lambda e, psS=psS, kc=kc, T=T: e.matmul(psS[:], kT[:, kc * 128:(kc + 1) * 128],
                                                       qT[:, T * 512:(T + 1) * 512], start=True, stop=True),
                 reads=["kT", "qT"], writes=[("psS", par)])
            q0 = max(0, kc - 4 * T)
            P.act(lambda e, psS=psS, par=par, q0=q0: e.activation(PT[par][:, q0 * 128:512], psS[:, q0 * 128:512], AF.Exp,
                                                              bias=biasS[:], scale=scale),
                  reads=[("psS", par), "biasS"], writes=[("PT", par)])
            if kc >= 4 * T:
                P.pool(lambda e, par=par, q0=q0: e.tensor_tensor(PT[par][:, q0 * 128:(q0 + 1) * 128],
                                                             PT[par][:, q0 * 128:(q0 + 1) * 128], tri[:], ALU.mult),
                       reads=[("PT", par), "tri"], writes=[("PT", par)])
            for q in range(q0, 4):
                qi = 4 * T + q
                jo = j % 4
                j += 1
                psO = banks[2 + jo][:, 0:129]
                P.pe(lambda e, psO=psO, par=par, q=q, kc=kc: e.matmul(psO, PT[par][:, q * 128:(q + 1) * 128], V1[:, kc, :],
                                                                   start=True, stop=True),
                     reads=[("PT", par), "V1"], writes=[("psO", jo)])
                if kc == 0:
                    P.dve(lambda e, psO=psO, qi=qi, kc=kc: e.tensor_scalar(acc[:, qi, :], psO, w[:, qi, kc:kc + 1], None,
                                                                        ALU.mult),
                          reads=[("psO", jo), "w"], writes=[("acc", qi)])
                else:
                    P.dve(lambda e, psO=psO, qi=qi, kc=kc: e.scalar_tensor_tensor(
                        acc[:, qi, :], psO, w[:, qi, kc:kc + 1], acc[:, qi, :], ALU.mult, ALU.add),
                        reads=[("psO", jo), "w", ("acc", qi)], writes=[("acc", qi)])
        if T >= 2:
            P.flush()
    rinv = P.sb([128, 32, 1], F32)
    P.flush()
    P.dve(lambda e: e.reciprocal(rinv[:], acc[:, :, 128:129]), reads=[("acc", qi) for qi in range(32)], writes=["rinv"])
    o16 = P.sb([128, 32, 128], BF16)
    outT = P.sb([128, S], BF16)
    for qi in range(32):
        P.dve(lambda e, qi=qi: e.tensor_scalar(o16[:, qi, :], acc[:, qi, 0:128], rinv[:, qi, 0:1], None, ALU.mult),
              reads=[("acc", qi), "rinv"], writes=[("o16", qi)])
    for T in range(8):
        psT = banks[6 + T % 2]
        for q in range(4):
            qi = 4 * T + q
            P.pe(lambda e, psT=psT, q=q, qi=qi: e.matmul(psT[:, q * 128:(q + 1) * 128], o16[:, qi, :], identb[:],
                                                       start=True, stop=True),
                 reads=[("o16", qi), "identb"], writes=[("psT", T % 2)])
        P.act(lambda e, psT=psT, T=T: e.copy(outT[:, T * 512:(T + 1) * 512], psT[:]), reads=[("psT", T % 2)],
              writes=[("outT", T)])
        P.dma(out_dram[:, T * 512:(T + 1) * 512], outT[:, T * 512:(T + 1) * 512], reads=[("outT", T)], q="sp")
    return P.finish()


def phase_rwkv(nc, fo, prm, cst, out_dram, dbg=None):
    P = Prog(nc)
    dcnt = [0, 0]

    def dump_t(t, key, T):
        if dbg is not None and T == 0:
            P.dma(dbg["t"][dcnt[0]], t[:], reads=[key])
            dcnt[0] += 1

    def dump_c(t, key, T, cl, w=128):
        if dbg is not None and T == 0 and cl == 0:
            P.dma(dbg["c"][dcnt[1]][:, 0:w], t[:] if len(t.shape) == 2 else t[:].rearrange("p a b -> p (a b)"), reads=[key])
            dcnt[1] += 1

    banks = P.banks(8)

    def slot(b, s, w=128):
        return banks[b][:, s * 128:s * 128 + w], ("s", b, s)

    def ld(shape, src, name, q="sp"):
        t = P.sb(shape, F32)
        P.dma(t[:], src, writes=[name], q=q)
        return t

    wup = ld([96, 128], prm["wup"], "wup")
    aup = ld([96, 128], prm["aup"], "aup")
    gup = ld([128, 2, 128], prm["gup"], "gup")
    vec = ld([128, 8], prm["vec"], "vec")
    blk = ld([128, 128], cst["blk"], "blk")
    ident = ld([128, 128], cst["identf"], "ident")
    hm = ld([128, 2], cst["hm"], "hm")
    Msu = ld([128, 128], cst["Msu"], "Msu")
    Msl = ld([128, 128], cst["Msl"], "Msl")
    Mst = ld([128, 64], cst["Mst"], "Mst")
    MstN = ld([128, 64], cst["MstN"], "MstN")
    rmask = ld([128, 512], cst["rmask"], "rmask")
    omka = P.sb([128, 1], F32)
    P.dve(lambda e: e.tensor_scalar(omka[:], vec[:, 3:4], -1.0, 1.0, ALU.mult, ALU.add), reads=["vec"], writes=["omka"])
    epsG = P.sb([128, 1], F32)
    P.pool(lambda e: e.memset(epsG[:], GN_EPS), writes=["epsG"])
    Mbd = P.sb([128, 128], F32)
    P.pool(lambda e: e.memset(Mbd[:], 0.0), writes=["Mbd"])
    hmb = hm[:].unsqueeze(2).broadcast_to([128, 2, 64])

    names = ["wr", "wk", "wv", "wd", "wa", "wg0", "wg1"]
    rows = {"wr": 128, "wk": 128, "wv": 128, "wd": 96, "wa": 96, "wg0": 128, "wg1": 128}
    inb = {n: [P.sb([128, 512], F32) for _ in range(2)] for n in names}

    def T32(n=1):
        return P.sb([128, 512], F32)

    th = T32(); lw = T32(); a_ = T32(); sw0 = T32(); sw1 = T32(); g_ = T32(); kkr = T32(); sq = T32()
    rn = T32(); kk = T32(); t1 = T32(); kp = T32(); beta = T32(); rk3 = T32(); bonus = T32(); cum = T32()
    tmpc = T32(); epos = T32(); eneg = T32(); eprev = T32(); kapt = T32(); ktl = T32(); btl = T32(); rtl = T32()
    yT = T32(); yc = T32(); ysq = T32(); grs = T32(); yn = T32()
    o16 = [P.sb([128, 512], BF16) for _ in range(2)]

    def C32():
        return P.sb([128, 128], F32)

    E = {nm: [C32() for _ in range(2)] for nm in ("kape", "kte", "bte", "ve")}
    Xs = [[C32() for _ in range(2)] for _ in range(2)]
    Xps = [[C32() for _ in range(2)] for _ in range(2)]
    Ys = [[C32() for _ in range(2)] for _ in range(2)]
    AkkS = [C32() for _ in range(2)]
    ArkS = [P.sb([128, 64], F32) for _ in range(2)]
    ArbS = [P.sb([128, 64], F32) for _ in range(2)]
    Vbd = [C32() for _ in range(2)]
    kapbd = [C32() for _ in range(2)]
    ktbd = [C32() for _ in range(2)]
    btbdN = [C32() for _ in range(2)]
    W0 = [C32() for _ in range(2)]
    Vhat = [C32() for _ in range(2)]
    kaph = [C32() for _ in range(2)]
    Ub = [C32() for _ in range(2)]
    Mtmp = C32()

    def flat(t):
        return t[:].rearrange("p a b -> p (a b)") if len(t.shape) == 3 else t[:]

    for T in range(_DBG.get("ntiles", 8)):
        par = T % 2
        ts = slice(T * 512, (T + 1) * 512)
        I = {}
        for n in names:
            I[n] = inb[n][par]
            P.dma(I[n][0:rows[n], :], fo[n][:, ts], writes=[("in", n, par)])
        r_, k_, v_ = I["wr"], I["wk"], I["wv"]
        ik = lambda n: ("in", n, par)
        P.act(lambda e, I=I: e.activation(th[0:96, :], I["wd"][0:96, :], AF.Tanh), reads=[ik("wd")], writes=["th"])
        P.pe(lambda e: e.matmul(banks[0][:], wup[:], th[0:96, :], start=True, stop=True), reads=["wup", "th"],
             writes=[("bank", 0)])
        P.act(lambda e: e.activation(lw[:], banks[0][:], AF.Sigmoid, bias=vec[:, 0:1]), reads=[("bank", 0), "vec"],
              writes=["lw"])
        P.dve(lambda e: e.tensor_scalar(lw[:], lw[:], -math.exp(-0.5), None, ALU.mult), reads=["lw"], writes=["lw"])
        P.pe(lambda e, I=I: e.matmul(banks[1][:], aup[:], I["wa"][0:96, :], start=True, stop=True),
             reads=["aup", ik("wa")], writes=[("bank", 1)])
        P.act(lambda e: e.activation(a_[:], banks[1][:], AF.Sigmoid, bias=vec[:, 1:2]), reads=[("bank", 1), "vec"],
              writes=["a"])
        P.act(lambda e, I=I: e.activation(sw0[:], I["wg0"][:], AF.Sigmoid), reads=[ik("wg0")], writes=["sw0"])
        P.act(lambda e, I=I: e.activation(sw1[:], I["wg1"][:], AF.Sigmoid), reads=[ik("wg1")], writes=["sw1"])
        P.pe(lambda e: e.matmul(banks[0][:], gup[:, 0, :], sw0[:], start=True, stop=False), reads=["gup", "sw0"],
             writes=[("bank", 0)])
        P.pe(lambda e: e.matmul(banks[0][:], gup[:, 1, :], sw1[:], start=False, stop=True), reads=["gup", "sw1"],
             writes=[("bank", 0)])
        P.act(lambda e: e.copy(g_[:], banks[0][:]), reads=[("bank", 0)], writes=["g"])
        P.dve(lambda e, k_=k_: e.tensor_scalar(kkr[:], k_[:], vec[:, 2:3], None, ALU.mult), reads=[ik("wk"), "vec"],
              writes=["kkr"])
        P.act(lambda e: e.activation(sq[:], kkr[:], AF.Square), reads=["kkr"], writes=["sq"])
        P.pe(lambda e: e.matmul(banks[1][:], blk[:], sq[:], start=True, stop=True), reads=["blk", "sq"],
             writes=[("bank", 1)])
        P.dve(lambda e: e.tensor_scalar(rn[:], banks[1][:], 1e-24, None, ALU.max), reads=[("bank", 1)], writes=["rn"])
        P.act(lambda e: e.activation(rn[:], rn[:], AF.Sqrt), reads=["rn"], writes=["rn"])
        P.dve(lambda e: e.reciprocal(rn[:], rn[:]), reads=["rn"], writes=["rn"])
        P.dve(lambda e: e.tensor_tensor(kk[:], kkr[:], rn[:], ALU.mult), reads=["kkr", "rn"], writes=["kk"])
        P.dve(lambda e: e.tensor_scalar(t1[:], a_[:], vec[:, 3:4], omka[:, 0:1], ALU.mult, ALU.add),
              reads=["a", "vec", "omka"], writes=["t1"])
        P.dve(lambda e, k_=k_: e.tensor_tensor(kp[:], k_[:], t1[:], ALU.mult), reads=[ik("wk"), "t1"], writes=["kp"])
        P.pool(lambda e: e.tensor_tensor(beta[:], kk[:], a_[:], ALU.mult), reads=["kk", "a"], writes=["beta"])
        P.pool(lambda e, r_=r_: e.tensor_tensor(rk3[:], r_[:], kp[:], ALU.mult), reads=[ik("wr"), "kp"], writes=["rk3"])
        P.pool(lambda e: e.tensor_scalar(rk3[:], rk3[:], vec[:, 4:5], None, ALU.mult), reads=["rk3", "vec"],
               writes=["rk3"])
        P.pe(lambda e: e.matmul(banks[0][:], blk[:], rk3[:], start=True, stop=True), reads=["blk", "rk3"],
             writes=[("bank", 0)])
        P.dve(lambda e, v_=v_: e.tensor_tensor(bonus[:], banks[0][:], v_[:], ALU.mult), reads=[("bank", 0), ik("wv")],
              writes=["bonus"])
        P.dve(lambda e: e.tensor_tensor_scan(cum[:], rmask[:], lw[:], 0.0, ALU.mult, ALU.add), reads=["rmask", "lw"],
              writes=["cum"])
        P.pool(lambda e: e.tensor_tensor(tmpc[:], cum[:], lw[:], ALU.subtract), reads=["cum", "lw"], writes=["tmpc"])
        P.act(lambda e: e.activation(epos[:], cum[:], AF.Exp), reads=["cum"], writes=["epos"])
        P.act(lambda e: e.activation(eneg[:], cum[:], AF.Exp, scale=-1.0), reads=["cum"], writes=["eneg"])
        P.act(lambda e: e.activation(eprev[:], tmpc[:], AF.Exp), reads=["tmpc"], writes=["eprev"])
        P.dve(lambda e: e.tensor_tensor(kapt[:], kk[:], eprev[:], ALU.mult), reads=["kk", "eprev"], writes=["kapt"])
        P.pool(lambda e: e.tensor_tensor(ktl[:], kp[:], eneg[:], ALU.mult), reads=["kp", "eneg"], writes=["ktl"])
        P.dve(lambda e: e.tensor_tensor(btl[:], beta[:], eneg[:], ALU.mult), reads=["beta", "eneg"], writes=["btl"])
        P.pool(lambda e, r_=r_: e.tensor_tensor(rtl[:], r_[:], epos[:], ALU.mult), reads=[ik("wr"), "epos"],
               writes=["rtl"])
        for nm_, t_ in (("lw", lw), ("a", a_), ("g", g_), ("kk", kk), ("kp", kp), ("beta", beta), ("bonus", bonus),
                        ("cum", cum), ("epos", epos), ("eneg", eneg), ("eprev", eprev), ("kapt", kapt), ("ktl", ktl),
                        ("btl", btl), ("rtl", rtl)):
            dump_t(t_, nm_, T)
        for cl in range(8 if _DBG.get("chunks", True) else 0):
            cp = cl % 2
            cs = slice(cl * 64, (cl + 1) * 64)

            def bc(x):
                return x[:, cs].unsqueeze(1).broadcast_to([128, 2, 64])

            srcs = {"kape": (kapt, "kapt"), "kte": (ktl, "ktl"), "bte": (btl, "btl"), "ve": (v_, ik("wv"))}
            for xi, nm in enumerate(("kape", "kte", "bte", "ve")):
                src, sk = srcs[nm]
                dst = E[nm][cp]
                eng = P.dve if xi % 2 == 0 else P.pool
                eng(lambda e, cp=cp, dst=dst, src=src, cs=cs: e.tensor_tensor(
                    dst[:].rearrange("p (a b) -> p a b", a=2), src[:, cs].unsqueeze(1).broadcast_to([128, 2, 64]),
                    hmb, ALU.mult), reads=[sk, "hm"], writes=[(nm, cp)])
            kape, kte, bte, ve = (E[nm][cp] for nm in ("kape", "kte", "bte", "ve"))
            pNp, kNp = slot(2, 0)
            pNn, kNn = slot(2, 1)
            pAkk, kAkk = slot(2, 2)
            pArk, kArk = slot(2, 3, 64)
            pArb = banks[2][:, 3 * 128 + 64:3 * 128 + 128]
            P.pe(lambda e, cp=cp, pNp=pNp, bte=bte, kape=kape: e.matmul(pNp, bte[:], kape[:], start=True, stop=True),
                 reads=[("bte", cp), ("kape", cp)], writes=[kNp])
            P.pe(lambda e, cp=cp, pNn=pNn, bte=bte, kape=kape: e.matmul(pNn, kape[:], bte[:], start=True, stop=True),
                 reads=[("bte", cp), ("kape", cp)], writes=[kNn])
            P.pe(lambda e, cp=cp, pAkk=pAkk, kte=kte, kape=kape: e.matmul(pAkk, kte[:], kape[:], start=True, stop=True),
                 reads=[("kte", cp), ("kape", cp)], writes=[kAkk])
            P.pe(lambda e, cp=cp, pArk=pArk, kte=kte, cs=cs: e.matmul(pArk, kte[:], rtl[:, cs], start=True, stop=True),
                 reads=[("kte", cp), "rtl"], writes=[kArk])
            P.pe(lambda e, cp=cp, pArb=pArb, bte=bte, cs=cs: e.matmul(pArb, bte[:], rtl[:, cs], start=True, stop=True),
                 reads=[("bte", cp), "rtl"], writes=[kArk])
            Xp = Xps[0][cp]
            X = Xs[0][cp]
            P.dve(lambda e, cp=cp, Xp=Xp, pNp=pNp: e.tensor_tensor(Xp[:], pNp, Msu[:], ALU.mult), reads=[kNp, "Msu"],
                  writes=[("Xp", 0, cp)])
            P.dve(lambda e, cp=cp, X=X, pNn=pNn: e.tensor_tensor(X[:], pNn, Msl[:], ALU.mult), reads=[kNn, "Msl"],
                  writes=[("X", 0, cp)])
            P.dve(lambda e, cp=cp, pAkk=pAkk: e.tensor_tensor(AkkS[cp][:], pAkk, Msu[:], ALU.mult), reads=[kAkk, "Msu"],
                  writes=[("AkkS", cp)])
            P.dve(lambda e, cp=cp, pArk=pArk: e.tensor_tensor(ArkS[cp][:], pArk, Mst[:], ALU.mult), reads=[kArk, "Mst"],
                  writes=[("ArkS", cp)])
            P.dve(lambda e, cp=cp, pArb=pArb: e.tensor_tensor(ArbS[cp][:], pArb, MstN[:], ALU.mult), reads=[kArk, "MstN"],
                  writes=[("ArbS", cp)])
            for si, (srcE, dstT, nm, neg) in enumerate(((ve, Vbd[cp], "Vbd", False), (kape, kapbd[cp], "kapbd", False),
                                                        (kte, ktbd[cp], "ktbd", False), (bte, btbdN[cp], "btbdN", True))):
                pT, kT_ = slot(3, si)
                srcn = ("ve", "kape", "kte", "bte")[si]
                P.pe(lambda e, cp=cp, pT=pT, srcE=srcE: e.matmul(pT, srcE[:], ident[:], start=True, stop=True),
                     reads=[(srcn, cp), "ident"], writes=[kT_])
                if neg:
                    P.act(lambda e, cp=cp, pT=pT, dstT=dstT: e.mul(dstT[:], pT, -1.0), reads=[kT_], writes=[(nm, cp)])
                else:
                    P.act(lambda e, cp=cp, pT=pT, dstT=dstT: e.copy(dstT[:], pT), reads=[kT_], writes=[(nm, cp)])
            Y = Ys[0][cp]
            P.pool(lambda e, cp=cp, Y=Y, Xp=Xp: e.tensor_tensor(Y[:], ident[:], Xp[:], ALU.subtract),
                   reads=["ident", ("Xp", 0, cp)], writes=[("Y", 0, cp)])
            ykey = ("Y", 0, cp)
            xkey = ("X", 0, cp)
            xpkey = ("Xp", 0, cp)
            for lvl in range(5):
                a1 = (lvl + 1) % 2
                last = lvl == 4
                pX, kX = slot(4, lvl % 4)
                pXp, kXp = slot(5, lvl % 4)
                Xn = Xs[a1][cp]
                Xpn = Xps[a1][cp]
                P.pe(lambda e, cp=cp, pX=pX, Xp=Xp, X=X: e.matmul(pX, Xp[:], X[:], start=True, stop=True),
                     reads=[xkey, xpkey], writes=[kX])
                P.act(lambda e, cp=cp, Xn=Xn, pX=pX: e.copy(Xn[:], pX), reads=[kX], writes=[("X", a1, cp)])
                if not last:
                    P.pe(lambda e, cp=cp, pXp=pXp, Xp=Xp, X=X: e.matmul(pXp, X[:], Xp[:], start=True, stop=True),
                         reads=[xkey, xpkey], writes=[kXp])
                    P.dve(lambda e, cp=cp, Xpn=Xpn, pXp=pXp: e.tensor_copy(Xpn[:], pXp), reads=[kXp], writes=[("Xp", a1, cp)])
                pY, kY = slot(6, lvl % 4)
                Yn = Ys[a1][cp]
                P.pe(lambda e, cp=cp, pY=pY, Xn=Xn, Y=Y: e.matmul(pY, Xn[:], Y[:], start=True, stop=True),
                     reads=[("X", a1, cp), ykey], writes=[kY])
                P.dve(lambda e, cp=cp, Yn=Yn, Y=Y, pY=pY: e.tensor_tensor(Yn[:], pY, Y[:], ALU.add), reads=[kY, ykey],
                      writes=[("Y", a1, cp)])
                X, Xp, Y = Xn, Xpn, Yn
                xkey, xpkey, ykey = ("X", a1, cp), ("Xp", a1, cp), ("Y", a1, cp)
            TT = Y
            dump_c(E["kape"][cp], ("kape", cp), T, cl)
            dump_c(E["ve"][cp], ("ve", cp), T, cl)
            dump_c(Xps[0][cp], ("Xp", 0, cp), T, cl)
            dump_c(Xs[0][cp], ("X", 0, cp), T, cl)
            dump_c(AkkS[cp], ("AkkS", cp), T, cl)
            dump_c(ArkS[cp], ("ArkS", cp), T, cl, 64)
            dump_c(ArbS[cp], ("ArbS", cp), T, cl, 64)
            dump_c(Vbd[cp], ("Vbd", cp), T, cl)
            dump_c(TT, ykey, T, cl)
            pW0, kW0 = slot(4, 0)
            pVh, kVh = slot(5, 0)
            pKh, kKh = slot(6, 0)
            P.pe(lambda e, cp=cp, pW0=pW0: e.matmul(pW0, AkkS[cp][:], Vbd[cp][:], start=True, stop=True),
                 reads=[("AkkS", cp), ("Vbd", cp)], writes=[kW0])
            P.act(lambda e, cp=cp, pW0=pW0: e.copy(W0[cp][:], pW0), reads=[kW0], writes=[("W0", cp)])
            P.pe(lambda e, cp=cp, pVh=pVh, TT=TT: e.matmul(pVh, TT[:], W0[cp][:], start=True, stop=True),
                 reads=[ykey, ("W0", cp)], writes=[kVh])
            P.act(lambda e, cp=cp, pVh=pVh: e.copy(Vhat[cp][:], pVh), reads=[kVh], writes=[("Vhat", cp)])
            P.pe(lambda e, cp=cp, pKh=pKh, TT=TT: e.matmul(pKh, kapbd[cp][:], TT[:], start=True, stop=True),
                 reads=[ykey, ("kapbd", cp)], writes=[kKh])
            P.dve(lambda e, cp=cp, pKh=pKh: e.tensor_copy(kaph[cp][:], pKh), reads=[kKh], writes=[("kaph", cp)])
            pU, kU = slot(7, 0)
            pM, kM = slot(7, 1)
            pYT, kYT = slot(7, 2, 64)
            P.pe(lambda e, cp=cp, pU=pU: e.matmul(pU, kaph[cp][:], Mbd[:], start=True, stop=True),
                 reads=[("kaph", cp), "Mbd"], writes=[kU])
            P.dve(lambda e, cp=cp, pU=pU: e.tensor_tensor(Ub[cp][:], pU, Vhat[cp][:], ALU.add), reads=[kU, ("Vhat", cp)],
                  writes=[("Ub", cp)])
            P.pe(lambda e, cp=cp, pYT=pYT, cs=cs: e.matmul(pYT, Mbd[:], rtl[:, cs], start=True, stop=False),
                 reads=["Mbd", "rtl"], writes=[kYT])
            P.pe(lambda e, cp=cp, pYT=pYT: e.matmul(pYT, Vbd[cp][:], ArkS[cp][:], start=False, stop=False),
                 reads=[("Vbd", cp), ("ArkS", cp)], writes=[kYT])
            P.pe(lambda e, cp=cp, pYT=pYT: e.matmul(pYT, Ub[cp][:], ArbS[cp][:], start=False, stop=True),
                 reads=[("Ub", cp), ("ArbS", cp)], writes=[kYT])
            P.act(lambda e, cp=cp, pYT=pYT, cs=cs: e.copy(yT[:, cs], pYT), reads=[kYT], writes=["yT"])
            P.pe(lambda e, cp=cp, pM=pM: e.matmul(pM, ktbd[cp][:], Vbd[cp][:], start=True, stop=False),
                 reads=[("ktbd", cp), ("Vbd", cp)], writes=[kM])
            P.pe(lambda e, cp=cp, pM=pM: e.matmul(pM, btbdN[cp][:], Ub[cp][:], start=False, stop=True),
                 reads=[("btbdN", cp), ("Ub", cp)], writes=[kM])
            P.dve(lambda e, cp=cp, pM=pM: e.tensor_tensor(Mtmp[:], pM, Mbd[:], ALU.add), reads=[kM, "Mbd"], writes=["Mtmp"])
            cend = cl * 64 + 63
            P.act(lambda e, cp=cp, cend=cend: e.activation(Mbd[:], Mtmp[:], AF.Copy, scale=epos[:, cend:cend + 1]),
                  reads=["Mtmp", "epos"], writes=["Mbd"])
        dump_t(yT, "yT", T)
        P.pe(lambda e: e.matmul(banks[7][:], blk[:], yT[:], start=True, stop=True), reads=["blk", "yT"],
             writes=[("bank", 7)])
        P.dve(lambda e: e.scalar_tensor_tensor(yc[:], banks[7][:], -1.0 / 64, yT[:], ALU.mult, ALU.add),
              reads=[("bank", 7), "yT"], writes=["yc"])
        P.act(lambda e: e.activation(ysq[:], yc[:], AF.Square), reads=["yc"], writes=["ysq"])
        P.pe(lambda e: e.matmul(banks[7][:], blk[:], ysq[:], start=True, stop=True), reads=["blk", "ysq"],
             writes=[("bank", 7)])
        P.act(lambda e: e.activation(grs[:], banks[7][:], AF.Sqrt, bias=epsG[:], scale=1.0 / 64),
              reads=[("bank", 7), "epsG"], writes=["grs"])
        P.dve(lambda e: e.reciprocal(grs[:], grs[:]), reads=["grs"], writes=["grs"])
        P.dve(lambda e: e.tensor_tensor(yn[:], yc[:], grs[:], ALU.mult), reads=["yc", "grs"], writes=["yn"])
        P.act(lambda e: e.activation(yn[:], yn[:], AF.Identity, bias=vec[:, 6:7], scale=vec[:, 5:6]),
              reads=["yn", "vec"], writes=["yn"])
        P.pool(lambda e: e.tensor_tensor(yn[:], yn[:], bonus[:], ALU.add), reads=["yn", "bonus"], writes=["yn"])
        P.dve(lambda e, par=par: e.tensor_tensor(o16[par][:], yn[:], g_[:], ALU.mult), reads=["yn", "g"],
              writes=[("o16", par)])
        P.dma(out_dram[:, ts], o16[par][:], reads=[("o16", par)], q="sp")
        P.flush()
    return P.finish()


NCORES = 8
_CACHE = {}


def _p16(v):
    return np.ascontiguousarray(v.reshape(-1, 128).T)


def _run(nc, in_maps):
    res = run_bass_kernel_spmd(nc, in_maps, core_ids=list(range(len(in_maps))))
    return res.results


def build_mod():
    nc = bass.Bass("TRN2", target_bir_lowering=False)
    io = IO(nc)
    cP = io.inp("cP", [128, 2, 16], F32)
    wada = io.inp("wada", [2, 128, 16, 1536], F32)
    bada = io.inp("bada", [128, 2, 12], F32)
    modS = io.out("modS", [128, 2, 2, 12], F32)
    phase_mod(nc, cP, wada, bada, modS)
    return nc


def launch_mod(c, w_ada, b_ada):
    nc = build_mod()
    cP = np.ascontiguousarray(np.stack([_p16(c[0]), _p16(c[1])], axis=1))
    maps = []
    for i in range(NCORES):
        cols = slice(i * 1536, (i + 1) * 1536)
        wa = np.ascontiguousarray(w_ada[:, :, cols].reshape(2, 16, 128, 1536).transpose(0, 2, 1, 3))
        ba = np.ascontiguousarray(np.stack([_p16(b_ada[l, cols]) for l in range(2)], axis=1))
        maps.append({"cP": cP, "wada": wa, "bada": ba})
    res = _run(nc, maps)
    modP = np.zeros((2, 2, 128, 96), np.float32)
    for i in range(NCORES):
        ms = res[i]["modS"]
        for l in range(2):
            for b in range(2):
                modP[l, b][:, i * 12:(i + 1) * 12] = ms[:, l, b, :]
    return modP


def build_T1():
    nc = bass.Bass("TRN2", target_bir_lowering=False)
    io = IO(nc)
    x = io.inp("x", [D, NT], F32)
    modP = io.inp("modP", [128, 96], F32)
    nrm = io.inp("nrm", [128, 16], F32)
    h = io.out("h", [D, NT], BF16)
    phase_norm(nc, x, NT, TILES, nrm, h, BF16, modP_dram=modP, sc_off=16, sh_off=0)
    return nc


def launch_T1(xT_parts, modP_l, norm_mix_l):
    nc = build_T1()
    nrm = _p16(norm_mix_l)
    maps = [{"x": xT_parts[i], "modP": np.ascontiguousarray(modP_l[i // 4]), "nrm": nrm} for i in range(NCORES)]
    res = _run(nc, maps)
    return [res[i]["h"] for i in range(NCORES)]


def build_T2(final):
    nc = bass.Bass("TRN2", target_bir_lowering=False)
    io = IO(nc)
    x = io.inp("x", [D, NTH], F32)
    mix = io.inp("mix", [D, NTH], BF16)
    wout = io.inp("wout", [128, KC, D], F32)
    modP = io.inp("modP", [128, 96], F32)
    nrm2 = io.inp("nrm2", [128, 16], F32)
    flag = io.inp("flag", [128, 1], F32)
    wup = io.inp("wup", [CP, 128, 2, KC, 128], F32)
    convP = io.inp("convP", [128, CP, 8], F32)
    wdn = io.inp("wdn", [KC, 128, CP, 128], F32)
    xout = io.out("xout", [D, NT], F32)
    xmid = io.scr("xmid", [D, NTH], F32)
    h2 = io.scr("h2", [D, NTH], BF16)
    phase_wout(nc, x, mix, wout, modP, xmid)
    phase_norm(nc, xmid, NTH, TILES_H, nrm2, h2, BF16, modP_dram=modP, sc_off=64, sh_off=48, flag_dram=flag, nhalo=NH)
    with nc.sbuf_tensor("aT_glob", [128, CP, NT], BF16) as aT:
        phase_ffn_up(nc, h2, wup, convP, aT)
        phase_ffn_down(nc, aT, wdn, xmid, modP, xout)
    if final:
        nrmf = io.inp("nrmf", [128, 16], F32)
        yout = io.out("yout", [D, NT], F32)
        phase_norm(nc, xout, NT, TILES, nrmf, yout, F32)
    return nc


def launch_T2(xT_parts, mixT_full, modP_l, p, l, final):
    nc = build_T2(final)
    wout = np.ascontiguousarray(p["w_out"][l].reshape(16, 128, D).transpose(1, 0, 2))
    wup = np.ascontiguousarray(p["w_ffn_up"][l].reshape(16, 128, 2, CP, 128).transpose(3, 1, 2, 0, 4))
    wdn = np.ascontiguousarray(p["w_ffn_down"][l].reshape(CP, 128, 16, 128).transpose(2, 1, 0, 3))
    cw = p["ffn_conv_w"][l].reshape(3, 2, CP, 128)
    cb = p["ffn_conv_b"][l].reshape(2, CP, 128)
    convP = np.zeros((128, CP, 8), np.float32)
    for half in range(2):
        for j in range(3):
            convP[:, :, half * 4 + j] = cw[j, half].T
        convP[:, :, half * 4 + 3] = cb[half].T
    nrm2 = _p16(p["norm_ffn"][l])
    maps = []
    for i in range(NCORES):
        b, j = i // 4, i % 4
        xh = np.zeros((D, NTH), np.float32)
        xh[:, NH:] = xT_parts[i]
        mh = np.zeros((D, NTH), NPBF)
        mh[:, NH:] = mixT_full[b][:, j * NT:(j + 1) * NT]
        if j > 0:
            xh[:, :NH] = xT_parts[i - 1][:, NT - NH:]
            mh[:, :NH] = mixT_full[b][:, j * NT - NH:j * NT]
        m = {"x": xh, "mix": mh, "wout": wout, "modP": np.ascontiguousarray(modP_l[b]), "nrm2": nrm2,
             "flag": np.full((128, 1), 0.0 if j == 0 else 1.0, np.float32), "wup": wup, "convP": convP, "wdn": wdn}
        if final:
            m["nrmf"] = _p16(p["norm_final"])
        maps.append(m)
    res = _run(nc, maps)
    xo = [res[i]["xout"] for i in range(NCORES)]
    yo = [res[i]["yout"] for i in range(NCORES)] if final else None
    return xo, yo


def _h_consts():
    if "h" in _CACHE:
        return _CACHE["h"]
    c = {}
    idx = np.arange(128)
    c["ret"] = []
    for h in range(4):
        lg = np.log1p(-np.exp2(-5.0 - h))
        diff = idx[None, :] - idx[:, None]
        decT = np.where(diff >= 0, np.exp(lg * np.maximum(diff, 0)), 0.0) * (64 ** -0.5)
        qdec = np.broadcast_to(np.exp(lg * (idx + 1.0))[None, :], (64, 128))
        kdec = (np.exp(lg * (127.0 - idx)) * (64 ** -0.5))[:, None]
        gC = np.full((64, 1), np.exp(lg * 128.0))
        c["ret"].append({"decT": decT.astype(np.float32), "qdec": np.ascontiguousarray(qdec).astype(np.float32),
                         "kdec": kdec.astype(np.float32), "gC": gC.astype(np.float32)})
    slopes = np.exp2(-8.0 * np.arange(1, 9) / 8.0)
    c["slopes"] = slopes
    tri = (idx[:, None] <= idx[None, :]).astype(np.float32)
    c["tri"] = tri.astype(NPBF)
    c["identb"] = np.eye(128, dtype=np.float32).astype(NPBF)
    negm = np.zeros((128, 32, 16), np.float32)
    ownm = np.zeros((128, 32, 16), np.float32)
    for qi in range(32):
        bq = qi // 2
        negm[:, qi, bq:] = -1e30
        ownm[:, qi, bq] = 1.0
    c["negm"] = negm
    c["ownm"] = ownm
    hd = idx // 64
    c["blk"] = (hd[:, None] == hd[None, :]).astype(np.float32)
    c["identf"] = np.eye(128, dtype=np.float32)
    hm = np.zeros((128, 2), np.float32)
    hm[:64, 0] = 1.0
    hm[64:, 1] = 1.0
    c["hm"] = hm
    loc = idx % 64
    same = hd[:, None] == hd[None, :]
    c["Msu"] = (same & (loc[:, None] < loc[None, :])).astype(np.float32)
    c["Msl"] = (same & (loc[:, None] > loc[None, :])).astype(np.float32)
    t64 = np.arange(64)
    c["Mst"] = (loc[:, None] <= t64[None, :]).astype(np.float32)
    c["MstN"] = -c["Mst"]
    rmask = np.ones((128, 512), np.float32)
    rmask[:, ::64] = 0.0
    c["rmask"] = rmask
    _CACHE["h"] = c
    return c


def _moba_tabs(heads):
    c = _h_consts()
    p = np.arange(128, dtype=np.float64)
    biasS = np.zeros((2, 128, 1), np.float32)
    Ew = np.zeros((2, 128, 32, 32), np.float32)
    for hh, h in enumerate(heads):
        sl = c["slopes"][h]
        biasS[hh, :, 0] = sl * (p - 127.0)
        for qi in range(32):
            for kc in range(qi + 1):
                Ew[hh, :, qi, kc] = np.exp(-sl * (128.0 * (qi - kc) + p - 127.0))
    return biasS, Ew


def build_H(phases=("inproj", "ret", "rwkv", "moba0", "moba1"), debug=False):
    nc = bass.Bass("TRN2", target_bir_lowering=False)
    io = IO(nc)
    has_in = "inproj" in phases

    def mid(name, shape, dt):
        if not has_in:
            return io.inp(name, shape, dt)
        if debug:
            return io.out(name, shape, dt)
        return io.scr(name, shape, dt)

    fo = {}
    for (name, ncol, r, dt) in FCH:
        fo[name] = mid("f_" + name, [ncol, S], dt)
    to = {"rv": mid("t_rv", [S, 128], BF16), "rkt": mid("t_rkt", [S, 64], BF16), "mv": mid("t_mv", [S, 256], BF16)}
    mix = io.out("mixp", [512, S], BF16)
    if has_in:
        hT = io.inp("hT", [D, S], BF16)
        wf = io.inp("wf", [128, KC, NF], F32)
        wt = io.inp("wt", [128, KC, NTK], F32)
        muP = io.inp("muP", [128, 7], F32)
        phase_inproj(nc, hT, wf, wt, muP, fo, to)
    if "ret" in phases:
        rc = {k: io.inp("rc_" + k, shp, F32) for k, shp in (("decT", [128, 128]), ("qdec", [64, 128]), ("kdec", [128, 1]), ("gC", [64, 1]))}
        phase_ret(nc, fo["rq"], fo["rk"], fo["rg"], to["rv"], to["rkt"], rc, mix[0:128, :])
    if "rwkv" in phases:
        wprm = {"wup": io.inp("w_wup", [96, 128], F32), "aup": io.inp("w_aup", [96, 128], F32),
                "gup": io.inp("w_gup", [128, 2, 128], F32), "vec": io.inp("w_vec", [128, 8], F32)}
        wc = {k: io.inp("wc_" + k, shp, F32) for k, shp in (("blk", [128, 128]), ("identf", [128, 128]), ("hm", [128, 2]),
                                                             ("Msu", [128, 128]), ("Msl", [128, 128]), ("Mst", [128, 64]),
                                                             ("MstN", [128, 64]), ("rmask", [128, 512]))}
        dbg = None
        if debug:
            dbg = {"t": io.out("dbg_t", [16, 128, 512], F32), "c": io.out("dbg_c", [9, 128, 128], F32)}
        phase_rwkv(nc, fo, wprm, wc, mix[128:256, :], dbg=dbg)
    if "moba0" in phases or "moba1" in phases:
        mc = {"biasS": io.inp("m_biasS", [2, 128, 1], F32), "Ew": io.inp("m_Ew", [2, 128, 32, 32], F32),
              "tri": io.inp("m_tri", [128, 128], BF16), "negm": io.inp("m_negm", [128, 32, 16], F32),
              "ownm": io.inp("m_ownm", [128, 32, 16], F32), "ident": io.inp("m_ident", [128, 128], BF16)}
        if "moba0" in phases:
            phase_moba(nc, fo["mq0"], fo["mk0"], to["mv"], 0, mc, mix[256:384, :])
        if "moba1" in phases:
            phase_moba(nc, fo["mq1"], fo["mk1"], to["mv"], 1, mc, mix[384:512, :])
    return nc, io


def _h_cols(j):
    RC, WC = 1536, 1984
    f = []
    f += list(range(64 * j, 64 * j + 64))
    f += list(range(256 + 64 * j, 256 + 64 * j + 64))
    f += list(range(1024 + 128 * j, 1024 + 128 * j + 128))
    w0 = RC
    f += list(range(w0 + 128 * j, w0 + 128 * j + 128))
    f += list(range(w0 + 512 + 128 * j, w0 + 512 + 128 * j + 128))
    f += list(range(w0 + 1024 + 128 * j, w0 + 1024 + 128 * j + 128))
    f += list(range(w0 + 1536, w0 + 1536 + 96))
    f += list(range(w0 + 1632, w0 + 1632 + 96))
    f += list(range(w0 + 1728, w0 + 1728 + 256))
    m0 = RC + WC
    for hh in range(2):
        h = 2 * j + hh
        f += list(range(m0 + 128 * h, m0 + 128 * h + 128))
    for hh in range(2):
        h = 2 * j + hh
        f += list(range(m0 + 1024 + 128 * h, m0 + 1024 + 128 * h + 128))
    t = []
    t += list(range(512 + 128 * j, 512 + 128 * j + 128))
    t += list(range(256 + 64 * j, 256 + 64 * j + 64))
    for hh in range(2):
        h = 2 * j + hh
        t += list(range(m0 + 2048 + 128 * h, m0 + 2048 + 128 * h + 128))
    return np.array(f), np.array(t)


def launch_H(hT_full, p, l):
    nc, io = build_H()
    c = _h_consts()
    maps = []
    win = p["w_in"][l]
    mu = p["rwkv_mu"][l]
    for i in range(NCORES):
        b, j = i // 4, i % 4
        fcols, tcols = _h_cols(j)
        wf = np.ascontiguousarray(win[:, fcols].reshape(16, 128, NF).transpose(1, 0, 2))
        wt = np.ascontiguousarray(win[:, tcols].reshape(16, 128, NTK).transpose(1, 0, 2))
        muP = np.zeros((128, 7), np.float32)
        ch = slice(128 * j, 128 * j + 128)
        muP[:, 0] = mu[0:512][ch]
        muP[:, 1] = mu[512:1024][ch]
        muP[:, 2] = mu[1024:1536][ch]
        muP[:96, 3] = mu[1536:1632]
        muP[:96, 4] = mu[1632:1728]
        muP[:, 5] = mu[1728:1856]
        muP[:, 6] = mu[1856:1984]
        m = {"hT": hT_full[b], "wf": wf, "wt": wt, "muP": muP}
        for k, v in c["ret"][j].items():
            m["rc_" + k] = v
        biasS, Ew = _moba_tabs([2 * j, 2 * j + 1])
        m.update({"m_biasS": biasS, "m_Ew": Ew, "m_tri": c["tri"], "m_negm": c["negm"], "m_ownm": c["ownm"],
                  "m_ident": c["identb"]})
        vec = np.zeros((128, 8), np.float32)
        vec[:, 0] = p["rwkv_w0"][l][ch]
        vec[:, 1] = p["rwkv_a0"][l][ch]
        vec[:, 2] = p["rwkv_k_k"][l][ch]
        vec[:, 3] = p["rwkv_k_a"][l][ch]
        vec[:, 4] = p["rwkv_r_k"][l].reshape(-1)[ch]
        vec[:, 5] = p["rwkv_ln_w"][l][ch]
        vec[:, 6] = p["rwkv_ln_b"][l][ch]
        m.update({"w_wup": np.ascontiguousarray(p["rwkv_w_up"][l][:, ch]), "w_aup": np.ascontiguousarray(p["rwkv_a_up"][l][:, ch]),
                  "w_gup": np.ascontiguousarray(p["rwkv_g_up"][l][:, ch].reshape(2, 128, 128).transpose(1, 0, 2)),
                  "w_vec": vec})
        for k in ("blk", "identf", "hm", "Msu", "Msl", "Mst", "MstN", "rmask"):
            m["wc_" + k] = c[k]
        maps.append({k: v for k, v in m.items() if k in io.ins})
    res = _run(nc, maps)
    mixT = [np.zeros((D, S), NPBF) for _ in range(B)]
    for i in range(NCORES):
        b, j = i // 4, i % 4
        mp = res[i]["mixp"]
        mixT[b][128 * j:128 * j + 128] = mp[0:128]
        mixT[b][512 + 128 * j:512 + 128 * j + 128] = mp[128:256]
        mixT[b][1024 + 256 * j:1024 + 256 * j + 256] = mp[256:512]
    return mixT


def kernel(**inputs):
    p = {k: np.asarray(v) for k, v in inputs.items()}
    x = p["x"]
    modP = launch_mod(p["c"], p["w_ada"], p["b_ada"])
    xT = [np.ascontiguousarray(x[i // 4, (i % 4) * NT:(i % 4 + 1) * NT].T) for i in range(NCORES)]
    yo = None
    for l in range(2):
        h = launch_T1(xT, modP[l], p["norm_mix"][l])
        hT_full = [np.ascontiguousarray(np.concatenate([h[b * 4 + j] for j in range(4)], axis=1)) for b in range(B)]
        mixT = launch_H(hT_full, p, l)
        xT, yo = launch_T2(xT, mixT, modP[l], p, l, final=(l == 1))
    out = np.zeros((B, S, D), np.float32)
    for i in range(NCORES):
        out[i // 4, (i % 4) * NT:(i % 4 + 1) * NT, :] = yo[i].T
    return out
```
